# Optimizing a Trainium2 kernel written in Bass

```python
import jax, jax.numpy as jnp
from jax import lax
import numpy as np

D_MODEL = 1024
BATCH = 8
SEQ = 8192
DEPTH = 1

MIX_WIDTH = D_MODEL
ATT_WIDTH = MIX_WIDTH // 2
ATT_HEAD_DIM = 64
ATT_HEADS = ATT_WIDTH // ATT_HEAD_DIM
KV_LATENT = 128
IDX_HEADS = 8
IDX_DIM = 64
TOPK_MAX = 256
Q_BLOCK = 128
HG_WIDTH = MIX_WIDTH - ATT_WIDTH
HG_HEADS = 8
HG_VAL_DIM = HG_WIDTH // HG_HEADS
HG_KEY_DIM = HG_VAL_DIM
HG_CHUNK = 32
PEER_HEADS = 8
PEER_N_KEYS = 128
PEER_N_EXPERTS = PEER_N_KEYS * PEER_N_KEYS
PEER_KEY_DIM = 256
PEER_TOPK = 16
PEER_BLOCK = 128
EPS = 1e-5
ALPHA = (2.0 * DEPTH) ** 0.25
BETA = (8.0 * DEPTH) ** -0.25

ATT_Q = ATT_HEADS * ATT_HEAD_DIM
HG_QK = HG_HEADS * HG_KEY_DIM
HG_V = HG_HEADS * HG_VAL_DIM
IN_SPLITS = (ATT_Q, KV_LATENT, IDX_HEADS * IDX_DIM, IDX_DIM, IDX_HEADS, HG_QK, HG_QK, HG_V, HG_V)
D_IN = ATT_Q + KV_LATENT + IDX_HEADS * IDX_DIM + IDX_DIM + IDX_HEADS + 2 * HG_QK + 2 * HG_V

kernel_name = 'hymba_dsa_hgrn2_peer_deepnorm'


def _layer_norm(x, g, b):
    xf = x.astype(jnp.float32)
    mu = jnp.mean(xf, axis=-1, keepdims=True)
    var = jnp.mean(jnp.square(xf - mu), axis=-1, keepdims=True)
    return ((xf - mu) * lax.rsqrt(var + EPS) * g + b).astype(x.dtype)


def _rms_norm(x, g):
    xf = x.astype(jnp.float32)
    return (xf * lax.rsqrt(jnp.mean(xf * xf, axis=-1, keepdims=True) + EPS) * g).astype(x.dtype)


def _split_columns(proj):
    points = [int(p) for p in np.cumsum(IN_SPLITS)[:-1]]
    return jnp.split(proj, points, axis=-1)


def _dsa_attention(q, c_kv, q_idx, k_idx, w_idx, w_uk, w_uv):
    B, L = q.shape[:2]
    top_k = min(TOPK_MAX, L // 4)
    nb = L // Q_BLOCK
    q_lat = jnp.einsum('blhd,hcd->blhc', q, w_uk) * (ATT_HEAD_DIM ** -0.5)
    key_pos = jnp.arange(L)

    def to_blocks(a):
        return jnp.moveaxis(a.reshape(B, nb, Q_BLOCK, *a.shape[2:]), 1, 0)

    def one_block(args):
        ql, qi, wi, qpos = args
        logits = jax.nn.relu(jnp.einsum('bqhd,bsd->bqhs', qi, k_idx))
        score = jnp.einsum('bqhs,bqh->bqs', logits, wi).astype(jnp.float32)
        causal = key_pos[None, :] <= qpos[:, None]
        score = jnp.where(causal[None], score, -jnp.inf)
        _, sel = lax.top_k(score, top_k)
        c_sel = jax.vmap(lambda c, i: c[i])(c_kv, sel)
        att = jnp.einsum('bqhc,bqkc->bqhk', ql, c_sel).astype(jnp.float32)
        valid = (sel <= qpos[None, :, None])[:, :, None, :]
        p = jax.nn.softmax(jnp.where(valid, att, -jnp.inf), axis=-1).astype(c_sel.dtype)
        return jnp.einsum('bqhk,bqkc->bqhc', p, c_sel)

    o_lat = lax.map(one_block, (to_blocks(q_lat), to_blocks(q_idx), to_blocks(w_idx),
                                jnp.arange(L).reshape(nb, Q_BLOCK)))
    o_lat = jnp.moveaxis(o_lat, 0, 1).reshape(B, L, ATT_HEADS, KV_LATENT)
    o = jnp.einsum('blhc,hcd->blhd', o_lat, w_uv)
    return o.reshape(B, L, ATT_Q)


def _hgrn2(q, f_logit, i, gate, lower_bound, g_norm):
    B, L = q.shape[:2]
    nc = L // HG_CHUNK
    f32 = jnp.float32
    lb = lower_bound.astype(f32)
    f = lb + (1.0 - lb) * jax.nn.sigmoid(f_logit.astype(f32))
    log_f = jnp.log(f)
    k = 1.0 - f
    qf = jax.nn.silu(q.astype(f32))

    def chunks(a, d):
        return a.reshape(B, nc, HG_CHUNK, HG_HEADS, d)

    qc, kc, gc = chunks(qf, HG_KEY_DIM), chunks(k, HG_KEY_DIM), chunks(log_f, HG_KEY_DIM)
    vc = chunks(i.astype(f32), HG_VAL_DIM)
    b = jnp.cumsum(gc, axis=2)
    b_last = b[:, :, -1:]
    q_t = qc * jnp.exp(b)
    k_t = kc * jnp.exp(-b)
    mask = jnp.tril(jnp.ones((HG_CHUNK, HG_CHUNK), dtype=bool))
    a = jnp.where(mask, jnp.einsum('bnthd,bnshd->bnhts', q_t, k_t), 0.0)
    o_intra = jnp.einsum('bnhts,bnshe->bnthe', a, vc)
    ds = jnp.einsum('bnshd,bnshe->bnhde', kc * jnp.exp(b_last - b), vc)
    decay = jnp.exp(b_last[:, :, 0])

    def step(s, inp):
        d, dsn = inp
        return d[..., None] * s + dsn, s

    s0 = jnp.zeros((B, HG_HEADS, HG_KEY_DIM, HG_VAL_DIM), f32)
    _, s_prev = lax.scan(step, s0, (jnp.moveaxis(decay, 1, 0), jnp.moveaxis(ds, 1, 0)))
    s_prev = jnp.moveaxis(s_prev, 0, 1)
    o_inter = jnp.einsum('bnthd,bnhde->bnthe', q_t, s_prev)
    o = (o_intra + o_inter).reshape(B, L, HG_HEADS, HG_VAL_DIM)
    o = _rms_norm(o, g_norm) * jax.nn.silu(gate.astype(f32).reshape(B, L, HG_HEADS, HG_VAL_DIM))
    return o.reshape(B, L, HG_V).astype(i.dtype)


def _peer(x, w_q, sub_keys, u, v):
    B, L, D = x.shape
    xt = x.reshape(-1, PEER_BLOCK, D)
    K = PEER_TOPK

    def one_block(xb):
        q = (xb @ w_q).reshape(PEER_BLOCK, PEER_HEADS, 2, PEER_KEY_DIM // 2)
        s = jnp.einsum('thpd,hpkd->thpk', q, sub_keys).astype(jnp.float32)
        s_top, i_top = lax.top_k(s, K)
        cand = s_top[:, :, 0, :, None] + s_top[:, :, 1, None, :]
        cand_idx = i_top[:, :, 0, :, None] * PEER_N_KEYS + i_top[:, :, 1, None, :]
        best, pos = lax.top_k(cand.reshape(PEER_BLOCK, PEER_HEADS, K * K), K)
        experts = jnp.take_along_axis(cand_idx.reshape(PEER_BLOCK, PEER_HEADS, K * K), pos, axis=-1)
        g = jax.nn.softmax(best, axis=-1).astype(xb.dtype)
        h = jax.nn.gelu(jnp.einsum('td,thkd->thk', xb, u[experts]))
        return jnp.einsum('thk,thkd->td', g * h, v[experts])

    return lax.map(one_block, xt).reshape(B, L, D)


def setup_inputs(seed: int = 0) -> dict:
    key = jax.random.key(seed)
    ks = jax.random.split(key, 16)
    f32 = jnp.float32

    def normal(k, shape, scale):
        return jax.random.normal(k, shape, f32) * scale

    hg_i_start = int(np.cumsum(IN_SPLITS)[6])
    col_scale = jnp.ones((D_IN,), f32).at[hg_i_start:hg_i_start + HG_V].set(BETA)
    return {
        'x': normal(ks[0], (BATCH, SEQ, D_MODEL), 1.0),
        'w_in': normal(ks[1], (DEPTH, D_MODEL, D_IN), D_MODEL ** -0.5) * col_scale,
        'kv_norm_g': 1.0 + normal(ks[2], (DEPTH, KV_LATENT), 0.02),
        'w_uk': normal(ks[3], (DEPTH, ATT_HEADS, KV_LATENT, ATT_HEAD_DIM), ATT_HEAD_DIM ** -0.5),
        'w_uv': normal(ks[4], (DEPTH, ATT_HEADS, KV_LATENT, ATT_HEAD_DIM), BETA * KV_LATENT ** -0.5),
        'hg_lb_logits': normal(ks[5], (DEPTH + 1, HG_QK), 0.5),
        'hg_norm_g': 1.0 + normal(ks[6], (DEPTH, HG_VAL_DIM), 0.02),
        'w_out': normal(ks[7], (DEPTH, MIX_WIDTH, D_MODEL), BETA * MIX_WIDTH ** -0.5),
        'ln1_g': 1.0 + normal(ks[8], (DEPTH, D_MODEL), 0.02),
        'ln1_b': normal(ks[9], (DEPTH, D_MODEL), 0.02),
        'peer_w_q': normal(ks[10], (DEPTH, D_MODEL, PEER_HEADS * PEER_KEY_DIM), D_MODEL ** -0.5),
        'peer_sub_keys': normal(ks[11], (DEPTH, PEER_HEADS, 2, PEER_N_KEYS, PEER_KEY_DIM // 2), (PEER_KEY_DIM // 2) ** -0.5),
        'peer_u': normal(ks[12], (DEPTH, PEER_N_EXPERTS, D_MODEL), BETA * D_MODEL ** -0.5),
        'peer_v': normal(ks[13], (DEPTH, PEER_N_EXPERTS, D_MODEL), BETA),
        'ln2_g': 1.0 + normal(ks[14], (DEPTH, D_MODEL), 0.02),
        'ln2_b': normal(ks[15], (DEPTH, D_MODEL), 0.02),
    }


def reference(x, w_in, kv_norm_g, w_uk, w_uv, hg_lb_logits, hg_norm_g, w_out, ln1_g, ln1_b,
              peer_w_q, peer_sub_keys, peer_u, peer_v, ln2_g, ln2_b):
    B, L, _ = x.shape
    lower_bounds = jnp.cumsum(jax.nn.softmax(hg_lb_logits.astype(jnp.float32), axis=0), axis=0)
    for layer in range(DEPTH):
        proj = x @ w_in[layer]
        q_att, c_kv, q_idx, k_idx, w_idx, hg_q, hg_f, hg_i, hg_gate = _split_columns(proj)
        att_out = _dsa_attention(
            q_att.reshape(B, L, ATT_HEADS, ATT_HEAD_DIM),
            _rms_norm(c_kv, kv_norm_g[layer]),
            q_idx.reshape(B, L, IDX_HEADS, IDX_DIM),
            k_idx, w_idx, w_uk[layer], w_uv[layer])
        hg_out = _hgrn2(hg_q, hg_f, hg_i, hg_gate, lower_bounds[layer], hg_norm_g[layer])
        mix = jnp.concatenate([att_out, hg_out], axis=-1) @ w_out[layer]
        h = _layer_norm(ALPHA * x + mix, ln1_g[layer], ln1_b[layer])
        ffn = _peer(h, peer_w_q[layer], peer_sub_keys[layer], peer_u[layer], peer_v[layer])
        x = _layer_norm(ALPHA * h + ffn, ln2_g[layer], ln2_b[layer])
    return x
```

```python
import numpy as np
from contextlib import ExitStack
import concourse.bass as bass
import concourse.mybir as mybir
from concourse.bass_utils import run_bass_kernel_spmd

F32 = mybir.dt.float32
BF16 = mybir.dt.bfloat16
I32 = mybir.dt.int32
U32 = mybir.dt.uint32
AF = mybir.ActivationFunctionType
ALU = mybir.AluOpType
AX = mybir.AxisListType

ENGS = ["sync", "scalar", "vector", "gpsimd", "tensor"]
SEG = 28000
DSEG = 28000

D = 1024
DIN = 3272
O_QA, O_KV, O_QI, O_KI, O_WI, O_HQ, O_HF, O_HI, O_HG = 0, 512, 640, 1152, 1216, 1224, 1736, 2248, 2760
EPS = 1e-5
ALPHA = 2.0 ** 0.25
TOPK = 256
NEG = -1.0e30


class Sched:
    def __init__(self, nc, es, tag=""):
        self.nc, self.es, self.tag = nc, SEM_ES[0], tag
        self.ops = {e: [] for e in ENGS}
        self.start = {e: 0 for e in ENGS}
        self.rank = {e: 0 for e in ENGS}
        self.esems = {e: [] for e in ENGS}
        self.dsems, self.dcnt, self.dgen = {}, {}, 0
        self.seen = {e: {} for e in ENGS}
        self.lastw, self.readers = {}, {}

    def _newsem(self, name):
        return self.es.enter_context(self.nc.semaphore(f"{self.tag}{name}"))

    def _dtoken(self, key):
        if key not in self.dsems or self.dcnt[key] + 16 > DSEG:
            self.dsems[key] = self._newsem(f"d_{key}_{self.dgen}")
            self.dgen += 1
            self.dcnt[key] = 0
        self.dcnt[key] += 16
        return ("d", self.dsems[key], self.dcnt[key])

    @staticmethod
    def _key(tok):
        return ("e", tok[1]) if tok[0] == "e" else ("d", id(tok[1]))

    def op(self, eng, fn, reads=(), writes=(), dma=None):
        deps = {}

        def add(tok):
            k = self._key(tok)
            if k not in deps or deps[k][2] < tok[2]:
                deps[k] = tok

        for r in reads:
            if r in self.lastw:
                add(self.lastw[r])
        for w in writes:
            if w in self.lastw:
                add(self.lastw[w])
            for t in self.readers.get(w, {}).values():
                add(t)
        idx = len(self.ops[eng])
        tok = ("e", eng, idx) if dma is None else self._dtoken(dma)
        waits = []
        for k, t in deps.items():
            if t[0] == "e":
                if t[1] == eng and eng == "tensor":
                    continue
                if t[2] < self.start[t[1]]:
                    continue
            if self.seen[eng].get(k, -1) >= t[2]:
                continue
            self.seen[eng][k] = t[2]
            if t[0] == "e":
                self.ops[t[1]][t[2]]["needed"] = True
            waits.append(t)
        self.ops[eng].append({"fn": fn, "waits": waits, "dtok": tok if dma is not None else None, "needed": False})
        kk = self._key(tok)
        for r in reads:
            d = self.readers.setdefault(r, {})
            if kk not in d or d[kk][2] < tok[2]:
                d[kk] = tok
        for w in writes:
            self.lastw[w] = tok
            self.readers[w] = {}
        return tok

    def final_wait(self, eng, keys):
        waits = [self.lastw[k] for k in keys if k in self.lastw and self.lastw[k][0] == "d"]
        self.ops[eng].append({"fn": None, "waits": waits, "dtok": None, "needed": False})

    def emit(self, block):
        for e in ENGS:
            for o in self.ops[e][self.start[e]:]:
                if o["needed"] and o["dtok"] is None:
                    k = self.rank[e]
                    self.rank[e] += 1
                    si = k // SEG
                    while len(self.esems[e]) <= si:
                        self.esems[e].append(self._newsem(f"e_{e}_{len(self.esems[e])}"))
                    o["sem"] = (self.esems[e][si], (k % SEG) + 1)

        def mk(ename):
            items = self.ops[ename][self.start[ename]:]

            def body(e):
                for o in items:
                    for t in o["waits"]:
                        if t[0] == "e":
                            sm, v = self.ops[t[1]][t[2]]["sem"]
                        else:
                            sm, v = t[1], t[2]
                        e.wait_ge(sm, v)
                    if o["fn"] is not None:
                        ins = o["fn"](e)
                        if o["dtok"] is not None:
                            ins.then_inc(o["dtok"][1], 16)
                        elif o["needed"]:
                            ins.then_inc(o["sem"][0], 1)

            return body

        block.sync(mk("sync"))
        block.scalar(mk("scalar"))
        block.vector(mk("vector"))
        block.gpsimd(mk("gpsimd"))
        block.tensor(mk("tensor"))
        for e in ENGS:
            self.start[e] = len(self.ops[e])


SEM_ES = [None]
STOP = 99
NTL = 10 ** 9
NTL3 = 10 ** 9
STOP3 = 99
GRP = [16, 4, 8]


def host_consts():
    c = {}
    p = np.arange(128)
    c["ident"] = np.eye(128, dtype=np.float32)
    same = (p[:, None] // 32) == (p[None, :] // 32)
    c["tcum"] = (same & (p[:, None] <= p[None, :])).astype(np.float32)
    c["tall"] = same.astype(np.float32)
    c["ind"] = ((p[:, None] // 32) == np.arange(4)[None, :]).astype(np.float32)
    cm = np.zeros((128, 4, 128), np.float32)
    for ch in range(4):
        cm[:, ch, ch * 32:(ch + 1) * 32] = 1.0
    c["cmask"] = cm.reshape(128, 512)
    cb = np.zeros((128, 4, 512), np.float32)
    sl = np.arange(512)
    for r in range(4):
        cb[:, r, :] = np.where(sl[None, :] <= 128 * r + p[:, None], 0.0, NEG)
    c["cbias"] = cb.reshape(128, 2048)
    c["halfs"] = np.tile((0.5 ** np.arange(1, 33))[None, :], (128, 1)).astype(np.float32)
    c["iota16"] = np.tile(np.arange(16, dtype=np.float32)[None, :], (128, 1))
    c["iota256"] = np.tile(np.arange(256, dtype=np.float32)[None, :], (128, 1))
    return c


CONST_SHAPES = {"ident": [128, 128], "tcum": [128, 128], "tall": [128, 128], "ind": [128, 4],
                "cmask": [128, 512], "cbias": [128, 2048], "halfs": [128, 32], "iota16": [128, 16],
                "iota256": [128, 256]}


def build(L, dbg=False, phases=(1, 2, 3)):
    NT = L // 128
    nc = bass.Bass("TRN2", target_bir_lowering=False)
    SEM_ES[0] = ExitStack()

    def din(name, shape, dt=F32):
        return nc.dram_tensor(name, shape, dt, kind="ExternalInput").ap()

    def dscr(name, shape, dt):
        return nc.dram_tensor(name, shape, dt, kind="ExternalOutput" if dbg else "Internal").ap()

    x = din("x", [L, D])
    w_in = din("w_in", [D, DIN])
    kv_norm_g = din("kv_norm_g", [1, 128])
    w_uk = din("w_uk", [8, 128, 64])
    w_uv = din("w_uv", [8, 128, 64])
    hg_lb_logits = din("hg_lb_logits", [2, 512])
    hg_norm_g = din("hg_norm_g", [1, 64])
    w_out = din("w_out", [D, D])
    ln1_g, ln1_b = din("ln1_g", [1, D]), din("ln1_b", [1, D])
    peer_w_q = din("peer_w_q", [D, 2048])
    peer_sub_keys = din("peer_sub_keys", [16, 128, 128])
    peer_u, peer_v = din("peer_u", [16384, D]), din("peer_v", [16384, D])
    ln2_g, ln2_b = din("ln2_g", [1, D]), din("ln2_b", [1, D])
    cst = {k: din("c_" + k, s) for k, s in CONST_SHAPES.items()}
    out = nc.dram_tensor("out", [L, D], F32, kind="ExternalOutput").ap()

    kT2_d = dscr("s_kT2", [128, L], BF16)
    qiT_d = dscr("s_qiT", [NT, 128, 8, 128], BF16)
    qlT_d = dscr("s_qlT", [NT, 128, 8, 128], BF16)
    cn_d = dscr("s_cn", [L, 128], BF16)
    cT_d = dscr("s_cT", [128, L], BF16)
    wi_d = dscr("s_wi", [L, 8], F32)
    mixT_d = dscr("s_mixT", [NT, 128, 8, 128], BF16)

    if 1 in phases:
      with ExitStack() as es:
        S = Sched(nc, es, "p1")
        sb = lambda n, s, d=F32: es.enter_context(nc.sbuf_tensor(n, s, d))
        ps = [es.enter_context(nc.psum_tensor(f"ps{k}", [128, 512], F32)) for k in range(8)]
        psb = lambda k: ps[k][:].bitcast(BF16)
        win = sb("win", [128, 8, DIN], BF16)
        wk2 = sb("wk2", [128, 8, 128], BF16)
        wukn = sb("wukn", [128, 8, 64], BF16)
        wukT = sb("wukT", [128, 8, 128], BF16)
        identb = sb("identb", [128, 128], BF16)
        identf = sb("identf", [128, 128], F32)
        tcum = sb("tcum", [128, 128], F32)
        tall = sb("tall", [128, 128], F32)
        ind = sb("ind", [128, 4], F32)
        cmask = sb("cmask", [128, 4, 128], F32)
        gkv = sb("gkv", [128, 128], F32)
        ghg = sb("ghg", [128, 64], F32)
        lbl = sb("lbl", [128, 2, 512], F32)
        lb = sb("lb", [128, 512], F32)
        oml = sb("oml", [128, 512], F32)
        epsb = sb("epsb", [128, 1], F32)
        xt = [sb(f"xt{b}", [128, D], F32) for b in range(2)]
        xb = sb("xb", [128, D], BF16)
        xT = sb("xT", [128, 8, 128], BF16)
        qT = sb("qT", [128, 4, 128], BF16)
        qiT = sb("qiT", [128, 8, 128], BF16)
        kT2 = sb("kT2", [128, 128], BF16)
        qlT = sb("qlT", [128, 8, 128], BF16)
        csq = sb("csq", [128, 128], F32)
        st1 = sb("st1", [128, 8], F32)
        st2 = sb("st2", [128, 8], F32)
        cn = sb("cn", [128, 128], BF16)
        cnf = sb("cnf", [128, 128], F32)
        cTs = sb("cTs", [128, 128], BF16)
        wis = sb("wis", [128, 8], F32)
        qf = sb("qf", [128, 512], F32)
        sg = sb("sg", [128, 512], F32)
        fg = sb("fg", [128, 512], F32)
        logf = sb("logf", [128, 512], F32)
        kk = sb("kk", [128, 512], F32)
        vb = sb("vb", [128, 512], BF16)
        sgate = sb("sgate", [128, 512], F32)
        eb = sb("eb", [128, 512], F32)
        enb = sb("enb", [128, 512], F32)
        ebl = sb("ebl", [128, 512], F32)
        qtb = sb("qtb", [128, 512], BF16)
        kt32 = sb("kt32", [128, 512], F32)
        ktb = sb("ktb", [128, 512], BF16)
        kpm = sb("kpm", [128, 4, 512], BF16)
        decT = sb("decT", [128, 4, 4], F32)
        qktT = sb("qktT", [128, 4, 128], BF16)
        kmT = sb("kmT", [128, 8, 128], BF16)
        qmT = sb("qmT", [128, 4, 4, 128], BF16)
        aT = sb("aT", [128, 8, 128], BF16)
        St = sb("St", [128, 4, 64], F32)
        Sb = sb("Sb", [128, 8, 64], BF16)
        oin = sb("oin", [128, 512], F32)
        osum = sb("osum", [128, 512], F32)
        osq = sb("osq", [128, 512], F32)
        hgo = sb("hgo", [128, 512], BF16)
        hgoT = sb("hgoT", [128, 4, 128], BF16)

        for G0 in range(0, min(NT, NTL), GRP[0]):
          with nc.Block() as block:
            if G0 == 0:
                for kc in range(8):
                    for c0 in range(0, DIN, 818):
                        S.op("gpsimd", lambda e, kc=kc, c0=c0: e.dma_start(out=win[:, kc, c0:c0 + 818], in_=w_in[kc * 128:(kc + 1) * 128, c0:c0 + 818]),
                             writes=["win"], dma="win")
                    for hh in range(2):
                        S.op("gpsimd", lambda e, kc=kc, hh=hh: e.dma_start(
                            out=wk2[:, kc, hh * 64:(hh + 1) * 64], in_=w_in[kc * 128:(kc + 1) * 128, O_KI:O_KI + 64]),
                            writes=["wk2"], dma="wk2")
                S.op("gpsimd", lambda e: e.dma_start(out=wukn[:], in_=w_uk.rearrange("h c d -> c h d")),
                     writes=["wukn"], dma="wukn")
                S.op("gpsimd", lambda e: e.dma_start(out=identb[:], in_=cst["ident"]), writes=["identb"], dma="identb")
                for nm, t in (("ident", identf), ("tcum", tcum), ("tall", tall), ("ind", ind)):
                    S.op("sync", lambda e, nm=nm, t=t: e.dma_start(out=t[:], in_=cst[nm]), writes=[t.name], dma="c_" + nm)
                S.op("sync", lambda e: e.dma_start(out=cmask[:].rearrange("p a b -> p (a b)"), in_=cst["cmask"]),
                     writes=["cmask"], dma="c_cmask")
                S.op("sync", lambda e: e.dma_start(out=gkv[:], in_=kv_norm_g[0].partition_broadcast(128)),
                     writes=["gkv"], dma="gkv")
                S.op("sync", lambda e: e.dma_start(out=ghg[:], in_=hg_norm_g[0].partition_broadcast(128)),
                     writes=["ghg"], dma="ghg")
                for r in range(2):
                    S.op("sync", lambda e, r=r: e.dma_start(out=lbl[:, r, :], in_=hg_lb_logits[r].partition_broadcast(128)),
                         writes=["lbl"], dma="lbl")
                S.op("vector", lambda e: e.tensor_tensor(out=oml[:], in0=lbl[:, 0, :], in1=lbl[:, 1, :], op=ALU.subtract),
                     reads=["lbl"], writes=["oml"])
                S.op("scalar", lambda e: e.activation(out=lb[:], in_=oml[:], func=AF.Sigmoid), reads=["oml"], writes=["lb"])
                S.op("vector", lambda e: e.tensor_scalar(out=oml[:], in0=lb[:], scalar1=-1.0, scalar2=1.0, op0=ALU.mult, op1=ALU.add),
                     reads=["lb"], writes=["oml"])
                S.op("vector", lambda e: e.memset(epsb[:], EPS), writes=["epsb"])
                S.op("vector", lambda e: e.memset(St[:], 0.0), writes=["St"])
                S.op("vector", lambda e: e.memset(Sb[:], 0.0), writes=["Sb"])
                S.op("vector", lambda e: e.memset(wukT[:], 0.0), writes=["wukT"])
                S.op("vector", lambda e: e.memset(qiT[:], 0.0), writes=["qiT"])
                S.op("vector", lambda e: e.memset(kmT[:], 0.0), writes=["kmT"])
                for j in range(4):
                    S.op("tensor", lambda e, j=j: e.transpose(
                        out=psb(0)[:, j * 128:(j + 1) * 128],
                        in_=wukn[:, 2 * j:2 * j + 2, :].rearrange("p a b -> p (a b)"), identity=identb[:]),
                        reads=["wukn", "identb"], writes=["ps0"])
                for hh in range(2):
                    S.op("vector", lambda e, hh=hh: e.tensor_copy(out=wukT[hh * 64:(hh + 1) * 64, hh:8:2, :],
                                                                  in_=psb(0)[hh * 64:(hh + 1) * 64, 0:512].rearrange("p (a b) -> p a b", b=128)),
                         reads=["ps0"], writes=["wukT"])

            for i in range(G0, min(G0 + GRP[0], NT, NTL)):
                t0 = i * 128
                b = i % 2
                xr = f"xt{b}"
                S.op("sync", lambda e, b=b, t0=t0: e.dma_start(out=xt[b][:], in_=x[t0:t0 + 128, :]), writes=[xr], dma=xr)
                S.op("scalar", lambda e, b=b: e.activation(out=xb[:], in_=xt[b][:], func=AF.Copy), reads=[xr], writes=["xb"])
                for kc in range(8):
                    S.op("tensor", lambda e, kc=kc: e.transpose(out=psb(0)[:, kc * 128:(kc + 1) * 128],
                                                                in_=xb[:, kc * 128:(kc + 1) * 128], identity=identb[:]),
                         reads=["xb", "identb"], writes=["ps0"])
                S.op("vector", lambda e: e.tensor_copy(out=xT[:].rearrange("p a b -> p (a b)"), in_=psb(0)),
                     reads=["ps0"], writes=["xT"])

                if STOP <= 1:
                    continue
                def mmgroup(out_ap, psname, fm, c0, ncol, lhs_tile=None):
                    for kc in range(8):
                        if fm:
                            l = (lhs_tile if lhs_tile is not None else win)[:, kc, c0:c0 + ncol]
                            r = xT[:, kc, :]
                        else:
                            l = xT[:, kc, :]
                            r = win[:, kc, c0:c0 + ncol]
                        S.op("tensor", lambda e, l=l, r=r, kc=kc: e.matmul(out_ap, lhsT=l, rhs=r, start=(kc == 0), stop=(kc == 7)),
                             reads=["xT", "win", "wk2"], writes=[psname])

                for j in range(4):
                    mmgroup(ps[1][:, j * 128:(j + 1) * 128], "ps1", True, O_QA + j * 128, 128)
                S.op("scalar", lambda e: e.activation(out=qT[:].rearrange("p a b -> p (a b)"), in_=ps[1][:], func=AF.Copy),
                     reads=["ps1"], writes=["qT"])
                for j in range(4):
                    mmgroup(ps[2][:, j * 128:(j + 1) * 128], "ps2", True, O_QI + j * 128, 128)
                for hh in range(2):
                    S.op("vector", lambda e, hh=hh: e.tensor_copy(out=qiT[hh * 64:(hh + 1) * 64, hh:8:2, :],
                                                                  in_=ps[2][hh * 64:(hh + 1) * 64, :].rearrange("p (a b) -> p a b", b=128)),
                         reads=["ps2"], writes=["qiT"])
                S.op("gpsimd", lambda e, t0=t0: e.dma_start(out=qiT_d[t0 // 128], in_=qiT[:]),
                     reads=["qiT"], writes=["d_qiT"], dma="st_qiT")
                mmgroup(ps[3][:, 0:128], "ps3", True, 0, 128, lhs_tile=wk2)
                mmgroup(ps[3][:, 128:256], "ps3", False, O_KV, 128)
                mmgroup(ps[3][:, 256:264], "ps3", False, O_WI, 8)
                S.op("scalar", lambda e: e.activation(out=kT2[:], in_=ps[3][:, 0:128], func=AF.Copy), reads=["ps3"], writes=["kT2"])
                S.op("gpsimd", lambda e, t0=t0: e.dma_start(out=kT2_d[:, t0:t0 + 128], in_=kT2[:]), reads=["kT2"], writes=["d_kT2"], dma="st_kT2")
                S.op("vector", lambda e: e.tensor_copy(out=wis[:], in_=ps[3][:, 256:264]), reads=["ps3"], writes=["wis"])
                S.op("gpsimd", lambda e, t0=t0: e.dma_start(out=wi_d[t0:t0 + 128, :], in_=wis[:]), reads=["wis"], writes=["d_wi"], dma="st_wi")
                if STOP <= 3:
                    continue
                S.op("scalar", lambda e: e.activation(out=csq[:], in_=ps[3][:, 128:256], func=AF.Square, accum_out=st1[:, 0:1]),
                     reads=["ps3"], writes=["csq", "st1"])
                S.op("scalar", lambda e: e.activation(out=st1[:, 1:2], in_=st1[:, 0:1], func=AF.Sqrt, scale=1.0 / 128, bias=epsb[:]),
                     reads=["st1", "epsb"], writes=["st1b"])
                S.op("vector", lambda e: e.reciprocal(out=st1[:, 2:3], in_=st1[:, 1:2]), reads=["st1b"], writes=["st1c"])
                S.op("vector", lambda e: e.tensor_scalar(out=cnf[:], in0=ps[3][:, 128:256], scalar1=st1[:, 2:3], scalar2=None, op0=ALU.mult),
                     reads=["ps3", "st1c"], writes=["cnf"])
                S.op("vector", lambda e: e.tensor_tensor(out=cn[:], in0=cnf[:], in1=gkv[:], op=ALU.mult),
                     reads=["cnf", "gkv"], writes=["cn"])
                S.op("gpsimd", lambda e, t0=t0: e.dma_start(out=cn_d[t0:t0 + 128, :], in_=cn[:]), reads=["cn"], writes=["d_cn"], dma="st_cn")
                S.op("tensor", lambda e: e.transpose(out=psb(3)[:, 0:128], in_=cn[:], identity=identb[:]),
                     reads=["cn", "identb"], writes=["ps3"])
                S.op("vector", lambda e: e.tensor_copy(out=cTs[:], in_=psb(3)[:, 0:128]), reads=["ps3"], writes=["cTs"])
                S.op("gpsimd", lambda e, t0=t0: e.dma_start(out=cT_d[:, t0:t0 + 128], in_=cTs[:]), reads=["cTs"], writes=["d_cT"], dma="st_cT")
                if STOP <= 4:
                    continue
                for h in range(8):
                    j, hh = h // 2, h % 2
                    pn = "ps1" if h < 4 else "ps2"
                    o = ps[1 if h < 4 else 2][:, (h % 4) * 128:(h % 4 + 1) * 128]
                    S.op("tensor", lambda e, o=o, j=j, hh=hh: e.matmul(o, lhsT=wukT[:, 2 * j + hh, :],
                                                                         rhs=qT[:, j, :], start=True, stop=True),
                         reads=["wukT", "qT"], writes=[pn])
                S.op("scalar", lambda e: e.activation(out=qlT[:, 0:4, :].rearrange("p a b -> p (a b)"), in_=ps[1][:], func=AF.Copy, scale=0.125),
                     reads=["ps1"], writes=["qlT"])
                S.op("scalar", lambda e: e.activation(out=qlT[:, 4:8, :].rearrange("p a b -> p (a b)"), in_=ps[2][:], func=AF.Copy, scale=0.125),
                     reads=["ps2"], writes=["qlT"])
                S.op("gpsimd", lambda e, t0=t0: e.dma_start(out=qlT_d[t0 // 128], in_=qlT[:]),
                     reads=["qlT"], writes=["d_qlT"], dma="st_qlT")

                if STOP <= 5:
                    continue
                mmgroup(ps[4][:], "ps4", False, O_HQ, 512)
                mmgroup(ps[5][:], "ps5", False, O_HF, 512)
                mmgroup(ps[6][:], "ps6", False, O_HI, 512)
                mmgroup(ps[7][:], "ps7", False, O_HG, 512)
                S.op("scalar", lambda e: e.activation(out=qf[:], in_=ps[4][:], func=AF.Silu), reads=["ps4"], writes=["qf"])
                S.op("scalar", lambda e: e.activation(out=sgate[:], in_=ps[7][:], func=AF.Silu), reads=["ps7"], writes=["sgate"])
                S.op("scalar", lambda e: e.activation(out=sg[:], in_=ps[5][:], func=AF.Sigmoid), reads=["ps5"], writes=["sg"])
                S.op("scalar", lambda e: e.activation(out=vb[:], in_=ps[6][:], func=AF.Copy), reads=["ps6"], writes=["vb"])
                S.op("vector", lambda e: e.tensor_tensor(out=fg[:], in0=sg[:], in1=oml[:], op=ALU.mult), reads=["sg", "oml"], writes=["fg"])
                S.op("vector", lambda e: e.tensor_tensor(out=fg[:], in0=fg[:], in1=lb[:], op=ALU.add), reads=["fg", "lb"], writes=["fg"])
                S.op("scalar", lambda e: e.activation(out=logf[:], in_=fg[:], func=AF.Ln), reads=["fg"], writes=["logf"])
                S.op("vector", lambda e: e.tensor_scalar(out=kk[:], in0=fg[:], scalar1=-1.0, scalar2=1.0, op0=ALU.mult, op1=ALU.add),
                     reads=["fg"], writes=["kk"])
                if STOP <= 6:
                    continue
                S.op("tensor", lambda e: e.matmul(ps[4][:], lhsT=tcum[:], rhs=logf[:], start=True, stop=True),
                     reads=["tcum", "logf"], writes=["ps4"])
                S.op("tensor", lambda e: e.matmul(ps[5][:], lhsT=tall[:], rhs=logf[:], start=True, stop=True),
                     reads=["tall", "logf"], writes=["ps5"])
                for j in range(4):
                    S.op("tensor", lambda e, j=j: e.matmul(ps[3][:, 256 + j * 4:256 + (j + 1) * 4], lhsT=logf[:, j * 128:(j + 1) * 128],
                                                           rhs=ind[:], start=True, stop=True),
                         reads=["logf", "ind"], writes=["ps3"])
                S.op("scalar", lambda e: e.activation(out=decT[:].rearrange("p a b -> p (a b)"), in_=ps[3][:, 256:272], func=AF.Exp),
                     reads=["ps3"], writes=["decT"])
                S.op("scalar", lambda e: e.activation(out=eb[:], in_=ps[4][:], func=AF.Exp), reads=["ps4"], writes=["eb"])
                S.op("scalar", lambda e: e.activation(out=enb[:], in_=ps[4][:], func=AF.Exp, scale=-1.0), reads=["ps4"], writes=["enb"])
                S.op("scalar", lambda e: e.activation(out=ebl[:], in_=ps[5][:], func=AF.Exp), reads=["ps5"], writes=["ebl"])
                S.op("vector", lambda e: e.tensor_tensor(out=qtb[:], in0=qf[:], in1=eb[:], op=ALU.mult), reads=["qf", "eb"], writes=["qtb"])
                S.op("vector", lambda e: e.tensor_tensor(out=kt32[:], in0=kk[:], in1=enb[:], op=ALU.mult), reads=["kk", "enb"], writes=["kt32"])
                S.op("vector", lambda e: e.tensor_copy(out=ktb[:], in_=kt32[:]), reads=["kt32"], writes=["ktb"])
                for c in range(4):
                    S.op("vector", lambda e, c=c: e.scalar_tensor_tensor(out=kpm[:, c, :], in0=kt32[:], scalar=ind[:, c:c + 1], in1=ebl[:],
                                                                         op0=ALU.mult, op1=ALU.mult),
                         reads=["kt32", "ebl", "ind"], writes=["kpm"])
                if STOP <= 7:
                    continue
                for j in range(4):
                    S.op("tensor", lambda e, j=j: e.transpose(out=psb(0)[:, j * 128:(j + 1) * 128], in_=qtb[:, j * 128:(j + 1) * 128], identity=identb[:]),
                         reads=["qtb", "identb"], writes=["ps0"])
                    S.op("tensor", lambda e, j=j: e.transpose(out=psb(0)[:, (4 + j) * 128:(5 + j) * 128], in_=ktb[:, j * 128:(j + 1) * 128], identity=identb[:]),
                         reads=["ktb", "identb"], writes=["ps0"])
                S.op("vector", lambda e: e.tensor_copy(out=qktT[:].rearrange("p a b -> p (a b)"), in_=psb(0)[:, 0:512]), reads=["ps0"], writes=["qktT"])
                for hh in range(2):
                    S.op("vector", lambda e, hh=hh: e.tensor_copy(out=kmT[hh * 64:(hh + 1) * 64, hh:8:2, :],
                                                                  in_=psb(0)[hh * 64:(hh + 1) * 64, 512:1024].rearrange("p (a b) -> p a b", b=128)),
                         reads=["ps0"], writes=["kmT"])
                if STOP <= 8:
                    continue
                S.op("gpsimd", lambda e: e.tensor_tensor(
                    out=qmT[:], in0=qktT[:].unsqueeze(2).to_broadcast([128, 4, 4, 128]),
                    in1=cmask[:].unsqueeze(1).to_broadcast([128, 4, 4, 128]), op=ALU.mult),
                    reads=["qktT", "cmask"], writes=["qmT"])
                if STOP <= 9:
                    continue
                for h in range(8):
                    j, hh = h // 2, h % 2
                    pk = 6 if h < 4 else 7
                    S.op("tensor", lambda e, j=j, hh=hh, pk=pk, h=h: e.matmul(
                        ps[pk][:, (h % 4) * 128:(h % 4 + 1) * 128], lhsT=kmT[:, h, :],
                        rhs=qktT[:, j, :], start=True, stop=True),
                        reads=["qktT", "kmT"], writes=[f"ps{pk}"])
                for half in range(2):
                    S.op("vector", lambda e, half=half: e.tensor_tensor(
                        out=aT[:, half * 4:(half + 1) * 4, :], in0=ps[6 + half][:].rearrange("p (a b) -> p a b", b=128),
                        in1=tcum[:].unsqueeze(1).to_broadcast([128, 4, 128]), op=ALU.mult),
                        reads=[f"ps{6 + half}", "tcum"], writes=["aT"])
                if STOP <= 10:
                    continue
                for h in range(8):
                    S.op("tensor", lambda e, h=h: e.matmul(ps[4][:, h * 64:(h + 1) * 64], lhsT=aT[:, h, :], rhs=vb[:, h * 64:(h + 1) * 64],
                                                           start=True, stop=True),
                         reads=["aT", "vb"], writes=["ps4"])
                S.op("scalar", lambda e: e.activation(out=oin[:], in_=ps[4][:], func=AF.Copy), reads=["ps4"], writes=["oin"])
                if STOP <= 11:
                    continue
                for c in range(4):
                    for h in range(8):
                        j, hh = h // 2, h % 2
                        S.op("tensor", lambda e, j=j, hh=hh, h=h, c=c: e.matmul(
                            ps[4 + c][:, h * 64:(h + 1) * 64], lhsT=qmT[:, j, c, :],
                            rhs=Sb[:, h, :], start=True, stop=True),
                            reads=["qmT", "Sb"], writes=[f"ps{4 + c}"])
                    for j in range(4):
                        S.op("tensor", lambda e, j=j, c=c: e.matmul(
                            ps[3][:, j * 128:(j + 1) * 128], lhsT=kpm[:, c, j * 128:(j + 1) * 128],
                            rhs=vb[:, j * 128:(j + 1) * 128], start=True, stop=True),
                            reads=["kpm", "vb"], writes=["ps3"])
                    for j in range(4):
                        for hh in range(2):
                            pr = slice(hh * 64, (hh + 1) * 64)
                            S.op("vector", lambda e, j=j, hh=hh, pr=pr, c=c: e.scalar_tensor_tensor(
                                out=St[pr, j, :], in0=St[pr, j, :], scalar=decT[pr, j, c:c + 1],
                                in1=ps[3][pr, j * 128 + hh * 64:j * 128 + (hh + 1) * 64], op0=ALU.mult, op1=ALU.add),
                                reads=["St", "decT", "ps3"], writes=["St"])
                    for hh in range(2):
                        S.op("scalar", lambda e, hh=hh: e.activation(out=Sb[hh * 64:(hh + 1) * 64, hh:8:2, :], in_=St[hh * 64:(hh + 1) * 64, :, :], func=AF.Copy),
                             reads=["St"], writes=["Sb"])
                if STOP <= 12:
                    continue
                S.op("vector", lambda e: e.tensor_tensor(out=osum[:], in0=ps[4][:], in1=oin[:], op=ALU.add), reads=["ps4", "oin"], writes=["osum"])
                for c in range(1, 4):
                    S.op("vector", lambda e, c=c: e.tensor_tensor(out=osum[:], in0=ps[4 + c][:], in1=osum[:], op=ALU.add),
                         reads=[f"ps{4 + c}", "osum"], writes=["osum"])
                S.op("gpsimd", lambda e: e.tensor_tensor(out=osq[:], in0=osum[:], in1=osum[:], op=ALU.mult), reads=["osum"], writes=["osq"])
                S.op("vector", lambda e: e.tensor_reduce(out=st2[:], in_=osq[:].rearrange("p (h e) -> p h e", e=64), axis=AX.X, op=ALU.add),
                     reads=["osq"], writes=["st2"])
                S.op("scalar", lambda e: e.activation(out=st2[:], in_=st2[:], func=AF.Sqrt, scale=1.0 / 64, bias=epsb[:]),
                     reads=["st2", "epsb"], writes=["st2"])
                S.op("vector", lambda e: e.reciprocal(out=st2[:], in_=st2[:]), reads=["st2"], writes=["st2"])
                S.op("vector", lambda e: e.tensor_tensor(out=osum[:].rearrange("p (h e) -> p h e", e=64), in0=osum[:].rearrange("p (h e) -> p h e", e=64),
                                                        in1=st2[:].unsqueeze(2).to_broadcast([128, 8, 64]), op=ALU.mult),
                     reads=["osum", "st2"], writes=["osum"])
                S.op("vector", lambda e: e.tensor_tensor(out=osum[:].rearrange("p (h e) -> p h e", e=64), in0=osum[:].rearrange("p (h e) -> p h e", e=64),
                                                        in1=ghg[:].unsqueeze(1).to_broadcast([128, 8, 64]), op=ALU.mult),
                     reads=["osum", "ghg"], writes=["osum"])
                S.op("vector", lambda e: e.tensor_tensor(out=hgo[:], in0=osum[:], in1=sgate[:], op=ALU.mult), reads=["osum", "sgate"], writes=["hgo"])
                for j in range(4):
                    S.op("tensor", lambda e, j=j: e.transpose(out=psb(0)[:, j * 128:(j + 1) * 128], in_=hgo[:, j * 128:(j + 1) * 128], identity=identb[:]),
                         reads=["hgo", "identb"], writes=["ps0"])
                S.op("vector", lambda e: e.tensor_copy(out=hgoT[:].rearrange("p a b -> p (a b)"), in_=psb(0)[:, 0:512]), reads=["ps0"], writes=["hgoT"])
                S.op("gpsimd", lambda e, t0=t0: e.dma_start(out=mixT_d[t0 // 128, :, 4:8, :], in_=hgoT[:]),
                     reads=["hgoT"], writes=["d_hgT"], dma="st_hgT")
            S.final_wait("sync", ["d_qiT", "d_kT2", "d_wi", "d_cn", "d_cT", "d_qlT", "d_hgT"])
            S.emit(block)

    if 2 in phases:
      with ExitStack() as es:
        S = Sched(nc, es, "p2")
        sb = lambda n, s, d=F32: es.enter_context(nc.sbuf_tensor(n, s, d))
        ps = [es.enter_context(nc.psum_tensor(f"qs{k}", [128, 512], F32)) for k in range(8)]
        psb = lambda k: ps[k][:].bitcast(BF16)
        NIT = 20
        kTa = sb("kTa", [128, L], BF16)
        cTa = sb("cTa", [128, L], BF16)
        caug = sb("caug", [128, NT, 129], BF16)
        score = sb("score", [128, L], F32)
        Mm = sb("Mm", [128, L], BF16)
        MT = sb("MT", [128, NT, 128], BF16)
        identb = sb("identb2", [128, 128], BF16)
        cbias = sb("cbias", [128, 4, 512], F32)
        halfs = sb("halfs", [128, 32], F32)
        wuvp = sb("wuvp", [128, 8, 128], BF16)
        qiT = [sb(f"qiTl{b}", [128, 8, 128], BF16) for b in range(2)]
        qlT = [sb(f"qlTl{b}", [128, 8, 128], BF16) for b in range(2)]
        wi = [sb(f"wil{b}", [128, 8], F32) for b in range(2)]
        Rr = [sb(f"Rr{b}", [128, 512], F32) for b in range(2)]
        Ee = [sb(f"Ee{b}", [128, 512], BF16) for b in range(2)]
        PT = [sb(f"PT{b}", [128, 512], BF16) for b in range(2)]
        bs = sb("bs", [128, 8], F32)
        hk = sb("hk", [128, 32], F32)
        rz = sb("rz", [128, 8], F32)
        olat = sb("olat", [128, 8, 128], BF16)
        olatT = sb("olatT", [128, 8, 128], BF16)
        attT = sb("attT", [128, 4, 128], BF16)
        for G0 in range(0, min(NT, NTL), GRP[1]):
          with nc.Block() as block:
            if G0 == 0:
                for q in range(0, L, 2048):
                    w = min(2048, L - q)
                    S.op("sync", lambda e, q=q, w=w: e.dma_start(out=kTa[:, q:q + w], in_=kT2_d[:, q:q + w]), writes=["kTa"], dma="kTa")
                    S.op("sync", lambda e, q=q, w=w: e.dma_start(out=cTa[:, q:q + w], in_=cT_d[:, q:q + w]), writes=["cTa"], dma="cTa")
                S.op("vector", lambda e: e.memset(caug[:], 1.0), writes=["caug"])
                for q in range(0, NT, 16):
                    w = min(16, NT - q)
                    S.op("sync", lambda e, q=q, w=w: e.dma_start(out=caug[:, q:q + w, 0:128],
                                                                  in_=cn_d[q * 128:(q + w) * 128, :].rearrange("(j p) c -> p j c", p=128)),
                         writes=["caug"], dma="caug")
                S.op("gpsimd", lambda e: e.dma_start(out=identb[:], in_=cst["ident"]), writes=["identb"], dma="identb2")
                S.op("sync", lambda e: e.dma_start(out=cbias[:].rearrange("p a b -> p (a b)"), in_=cst["cbias"]), writes=["cbias"], dma="cbias")
                S.op("sync", lambda e: e.dma_start(out=halfs[:], in_=cst["halfs"]), writes=["halfs"], dma="halfs")
                S.op("vector", lambda e: e.memset(wuvp[:], 0.0), writes=["wuvp"])
                for h in range(8):
                    S.op("gpsimd", lambda e, h=h: e.dma_start(out=wuvp[:, h, (h % 2) * 64:(h % 2 + 1) * 64], in_=w_uv[h]), writes=["wuvp"], dma="wuvp")
                sc_rot = 0
            for i in range(G0, min(G0 + GRP[1], NT, NTL)):
                t0 = i * 128
                b = i % 2
                nblk = i + 1
                nch = (nblk + 3) // 4
                nw = nch * 512 if nch * 512 <= L else L
                r = i % 4
                S.op("sync", lambda e, b=b, t0=t0: e.dma_start(out=qiT[b][:], in_=qiT_d[t0 // 128]),
                     writes=[f"qiT{b}"], dma=f"qiT{b}")
                S.op("sync", lambda e, b=b, t0=t0: e.dma_start(out=qlT[b][:], in_=qlT_d[t0 // 128]),
                     writes=[f"qlT{b}"], dma=f"qlT{b}")
                S.op("sync", lambda e, b=b, t0=t0: e.dma_start(out=wi[b][:], in_=wi_d[t0:t0 + 128, :]), writes=[f"wi{b}"], dma=f"wi{b}")
                for ci in range(nch):
                    c0 = ci * 512
                    cw = min(512, L - c0)
                    diag = (ci == nch - 1)
                    for h in range(8):
                        pk = sc_rot % 3
                        rb = sc_rot % 2
                        sc_rot += 1
                        S.op("tensor", lambda e, pk=pk, b=b, h=h, c0=c0, cw=cw: e.matmul(ps[pk][:, 0:cw], lhsT=qiT[b][:, h, :], rhs=kTa[:, c0:c0 + cw],
                                                                                       start=True, stop=True),
                             reads=[f"qiT{b}", "kTa"], writes=[f"ps{pk}"])
                        S.op("scalar", lambda e, pk=pk, rb=rb, cw=cw: e.activation(out=Rr[rb][:, 0:cw], in_=ps[pk][:, 0:cw], func=AF.Relu),
                             reads=[f"ps{pk}"], writes=[f"Rr{rb}"])
                        if h == 0:
                            if diag:
                                S.op("vector", lambda e, rb=rb, b=b, c0=c0, cw=cw, r=r: e.scalar_tensor_tensor(
                                    out=score[:, c0:c0 + cw], in0=Rr[rb][:, 0:cw], scalar=wi[b][:, 0:1], in1=cbias[:, r, 0:cw], op0=ALU.mult, op1=ALU.add),
                                    reads=[f"Rr{rb}", f"wi{b}", "cbias"], writes=["score"])
                            else:
                                S.op("vector", lambda e, rb=rb, b=b, c0=c0, cw=cw: e.tensor_scalar(
                                    out=score[:, c0:c0 + cw], in0=Rr[rb][:, 0:cw], scalar1=wi[b][:, 0:1], scalar2=None, op0=ALU.mult),
                                    reads=[f"Rr{rb}", f"wi{b}"], writes=["score"])
                        else:
                            S.op("vector", lambda e, rb=rb, b=b, c0=c0, cw=cw, h=h: e.scalar_tensor_tensor(
                                out=score[:, c0:c0 + cw], in0=Rr[rb][:, 0:cw], scalar=wi[b][:, h:h + 1], in1=score[:, c0:c0 + cw], op0=ALU.mult, op1=ALU.add),
                                reads=[f"Rr{rb}", f"wi{b}", "score"], writes=["score"])
                if nblk * 128 > TOPK:
                    S.op("vector", lambda e, nw=nw: e.tensor_reduce(out=bs[:, 0:1], in_=score[:, 0:nw], axis=AX.X, op=ALU.max), reads=["score"], writes=["bs_hi"])
                    S.op("vector", lambda e, i=i: e.tensor_reduce(out=bs[:, 1:2], in_=score[:, 0:128 * i], axis=AX.X, op=ALU.min), reads=["score"], writes=["bs_lo"])
                    S.op("vector", lambda e: e.tensor_tensor(out=bs[:, 2:3], in0=bs[:, 0:1], in1=bs[:, 1:2], op=ALU.subtract), reads=["bs_hi", "bs_lo"], writes=["bs_w"])
                    S.op("vector", lambda e: e.tensor_scalar(out=hk[:, 0:NIT], in0=halfs[:, 0:NIT], scalar1=bs[:, 2:3], scalar2=None, op0=ALU.mult),
                         reads=["bs_w", "halfs"], writes=["hk"])
                    for k in range(NIT):
                        S.op("vector", lambda e, k=k: e.tensor_tensor(out=bs[:, 3:4], in0=bs[:, 1:2], in1=hk[:, k:k + 1], op=ALU.add),
                             reads=["bs_lo", "hk"], writes=["bs_mid"])
                        S.op("vector", lambda e, nw=nw: e.tensor_scalar(out=Mm[:, 0:nw], in0=score[:, 0:nw], scalar1=bs[:, 3:4], scalar2=0.0,
                                                                         op0=ALU.is_ge, op1=ALU.add, accum_out=bs[:, 4:5]),
                             reads=["score", "bs_mid"], writes=["Mm", "bs_cnt"])
                        S.op("vector", lambda e, k=k: e.tensor_scalar(out=bs[:, 5:6], in0=bs[:, 4:5], scalar1=TOPK - 0.5, scalar2=hk[:, k:k + 1],
                                                                       op0=ALU.is_ge, op1=ALU.mult),
                             reads=["bs_cnt", "hk"], writes=["bs_tmp"])
                        S.op("vector", lambda e: e.tensor_tensor(out=bs[:, 1:2], in0=bs[:, 1:2], in1=bs[:, 5:6], op=ALU.add),
                             reads=["bs_lo", "bs_tmp"], writes=["bs_lo"])
                    S.op("vector", lambda e, nw=nw: e.tensor_scalar(out=Mm[:, 0:nw], in0=score[:, 0:nw], scalar1=bs[:, 1:2], scalar2=None, op0=ALU.is_ge),
                         reads=["score", "bs_lo"], writes=["Mm"])
                else:
                    S.op("vector", lambda e, nw=nw: e.tensor_scalar(out=Mm[:, 0:nw], in0=score[:, 0:nw], scalar1=-1.0e29, scalar2=None, op0=ALU.is_ge),
                         reads=["score"], writes=["Mm"])
                for g0 in range(0, nblk, 8):
                    gn = min(8, nblk - g0)
                    for k in range(gn):
                        jb = g0 + k
                        S.op("tensor", lambda e, jb=jb, k=k: e.transpose(out=psb(3)[:, k * 128:(k + 1) * 128], in_=Mm[:, jb * 128:(jb + 1) * 128], identity=identb[:]),
                             reads=["Mm", "identb"], writes=["ps3"])
                    S.op("scalar", lambda e, g0=g0, gn=gn: e.activation(out=MT[:, g0:g0 + gn, :].rearrange("p a b -> p (a b)"), in_=psb(3)[:, 0:gn * 128], func=AF.Copy),
                         reads=["ps3"], writes=["MT"])
                rot = 0
                for h in range(8):
                    po = 6 + (h % 2)
                    for g0 in range(0, nblk, 4):
                        gn = min(4, nblk - g0)
                        pq = 4 + (rot % 2)
                        eb_ = rot % 2
                        rot += 1
                        for k in range(gn):
                            jb = g0 + k
                            S.op("tensor", lambda e, pq=pq, k=k, jb=jb, b=b, h=h: e.matmul(ps[pq][:, k * 128:(k + 1) * 128], lhsT=cTa[:, jb * 128:(jb + 1) * 128],
                                                                                         rhs=qlT[b][:, h, :], start=True, stop=True),
                                 reads=["cTa", f"qlT{b}"], writes=[f"ps{pq}"])
                        S.op("scalar", lambda e, pq=pq, eb_=eb_, gn=gn: e.activation(out=Ee[eb_][:, 0:gn * 128], in_=ps[pq][:, 0:gn * 128], func=AF.Exp),
                             reads=[f"ps{pq}"], writes=[f"Ee{eb_}"])
                        S.op("gpsimd", lambda e, eb_=eb_, g0=g0, gn=gn: e.tensor_tensor(out=PT[eb_][:, 0:gn * 128], in0=Ee[eb_][:, 0:gn * 128],
                                                                                         in1=MT[:, g0:g0 + gn, :].rearrange("p a b -> p (a b)"), op=ALU.mult),
                             reads=[f"Ee{eb_}", "MT"], writes=[f"PT{eb_}"])
                        for k in range(gn):
                            jb = g0 + k
                            S.op("tensor", lambda e, po=po, eb_=eb_, k=k, jb=jb, i=i: e.matmul(ps[po][:, 0:129], lhsT=PT[eb_][:, k * 128:(k + 1) * 128], rhs=caug[:, jb, :],
                                                                                             start=(jb == 0), stop=(jb == i)),
                                 reads=[f"PT{eb_}", "caug"], writes=[f"ps{po}"])
                    S.op("vector", lambda e, po=po, h=h: e.reciprocal(out=rz[:, h:h + 1], in_=ps[po][:, 128:129]), reads=[f"ps{po}"], writes=[f"rz{h}"])
                    S.op("vector", lambda e, po=po, h=h: e.tensor_scalar(out=olat[:, h, :], in0=ps[po][:, 0:128], scalar1=rz[:, h:h + 1], scalar2=None, op0=ALU.mult),
                         reads=[f"ps{po}", f"rz{h}"], writes=["olat"])
                for h in range(8):
                    S.op("tensor", lambda e, h=h: e.transpose(out=psb(3)[:, h * 128:(h + 1) * 128], in_=olat[:, h, :], identity=identb[:]),
                         reads=["olat", "identb"], writes=["ps3"])
                S.op("scalar", lambda e: e.activation(out=olatT[:].rearrange("p a b -> p (a b)"), in_=psb(3), func=AF.Copy), reads=["ps3"], writes=["olatT"])
                for h in range(8):
                    j, hh = h // 2, h % 2
                    S.op("tensor", lambda e, h=h, j=j, hh=hh: e.matmul(ps[0][:, j * 128:(j + 1) * 128], lhsT=wuvp[:, h, :], rhs=olatT[:, h, :],
                                                                       start=(hh == 0), stop=(hh == 1)),
                         reads=["wuvp", "olatT"], writes=["ps0"])
                S.op("vector", lambda e: e.tensor_copy(out=attT[:].rearrange("p a b -> p (a b)"), in_=ps[0][:]), reads=["ps0"], writes=["attT"])
                S.op("sync", lambda e, t0=t0: e.dma_start(out=mixT_d[t0 // 128, :, 0:4, :], in_=attT[:]),
                     reads=["attT"], writes=["d_attT"], dma="st_attT")
            S.final_wait("sync", ["d_attT"])
            S.emit(block)

    if 3 in phases:
      with ExitStack() as es:
        S = Sched(nc, es, "p3")
        sb = lambda n, s, d=F32: es.enter_context(nc.sbuf_tensor(n, s, d))
        ps = [es.enter_context(nc.psum_tensor(f"rs{k}", [128, 512], F32)) for k in range(8)]
        psb = lambda k: ps[k][:].bitcast(BF16)
        wout = sb("wout", [128, 8, D], BF16)
        wq = sb("wq", [128, 8, 2048], BF16)
        skn = sb("skn", [128, 16, 128], BF16)
        skT = sb("skT", [128, 16, 128], BF16)
        identb = sb("identb3", [128, 128], BF16)
        iota16 = sb("iota16", [128, 16], F32)
        thr16 = sb("thr16", [128, 16], F32)
        lnp = sb("lnp", [128, 4, D], F32)
        epsb = sb("epsb3", [128, 1], F32)
        mixT = [sb(f"mixTl{b}", [128, 8, 128], BF16) for b in range(2)]
        xt = [sb(f"xt3{b}", [128, D], F32) for b in range(2)]
        hpre = sb("hpre", [128, D], F32)
        junk = sb("junk3", [128, D], F32)
        hres = sb("hres", [128, D], F32)
        hb = sb("hb", [128, D], BF16)
        hT = sb("hT", [128, 8, 128], BF16)
        qTs = sb("qTs", [128, 16, 128], BF16)
        ssb = sb("ssb", [128, 16, 128], F32)
        ss2 = sb("ss2", [128, 16, 128], F32)
        tv = sb("tv", [128, 16, 16], F32)
        ti = sb("ti", [128, 16, 16], U32)
        tif = sb("tif", [128, 16, 16], F32)
        cand = sb("cand", [128, 8, 256], F32)
        cand2 = ss2[:].rearrange("p a b -> p (a b)").rearrange("p (h c) -> p h c", c=256)
        best = sb("best", [128, 8, 16], F32)
        posu = sb("posu", [128, 8, 16], U32)
        posf = sb("posf", [128, 8, 16], F32)
        big = ssb[:].rearrange("p a b -> p (a b)").rearrange("p (h r a) -> p h r a", r=16, a=16)
        af = sb("af", [128, 8, 16], F32)
        bf_ = sb("bf_", [128, 8, 16], F32)
        ea = sb("ea", [128, 8, 16], F32)
        eb2 = sb("eb2", [128, 8, 16], F32)
        eidx = sb("eidx", [128, 128], U32)
        gg = sb("gg", [128, 8, 16], F32)
        gs = sb("gs", [128, 8], F32)
        hd = sb("hd", [128, 128], F32)
        g1 = sb("g1", [128, 128], F32)
        g2 = sb("g2", [128, 128], F32)
        wgt = sb("wgt", [128, 128], F32)
        st = sb("st3", [128, 8], F32)
        NB = 2
        ub = [sb(f"ub{k}", [128, D], F32) for k in range(NB)]
        acc = hpre
        yo = sb("yo", [128, D], F32)

        def layer_norm(src, dst, gi, tag):
            S.op("scalar", lambda e: e.activation(out=junk[:], in_=src[:], func=AF.Copy, accum_out=st[:, 0:1]), reads=[src.name], writes=["junk", "st_a"])
            S.op("vector", lambda e: e.tensor_scalar(out=st[:, 1:2], in0=st[:, 0:1], scalar1=-1.0 / D, scalar2=None, op0=ALU.mult), reads=["st_a"], writes=["st_b"])
            S.op("vector", lambda e: e.tensor_scalar(out=src[:], in0=src[:], scalar1=st[:, 1:2], scalar2=None, op0=ALU.add), reads=[src.name, "st_b"], writes=[src.name])
            S.op("scalar", lambda e: e.activation(out=junk[:], in_=src[:], func=AF.Square, accum_out=st[:, 2:3]), reads=[src.name], writes=["junk", "st_c"])
            S.op("scalar", lambda e: e.activation(out=st[:, 3:4], in_=st[:, 2:3], func=AF.Sqrt, scale=1.0 / D, bias=epsb[:]), reads=["st_c", "epsb"], writes=["st_d"])
            S.op("vector", lambda e: e.reciprocal(out=st[:, 4:5], in_=st[:, 3:4]), reads=["st_d"], writes=["st_e"])
            S.op("vector", lambda e: e.scalar_tensor_tensor(out=dst[:], in0=src[:], scalar=st[:, 4:5], in1=lnp[:, 2 * gi, :], op0=ALU.mult, op1=ALU.mult),
                 reads=[src.name, "st_e", "lnp"], writes=[dst.name])
            S.op("vector", lambda e: e.tensor_tensor(out=dst[:], in0=dst[:], in1=lnp[:, 2 * gi + 1, :], op=ALU.add), reads=[dst.name, "lnp"], writes=[dst.name])

        for G0 in range(0, min(NT, NTL, NTL3), GRP[2]):
          with nc.Block() as block:
            if G0 == 0:
                for kc in range(8):
                    S.op("gpsimd", lambda e, kc=kc: e.dma_start(out=wout[:, kc, :], in_=w_out[kc * 128:(kc + 1) * 128, :]), writes=["wout"], dma="wout")
                    for c0 in range(0, 2048, 1024):
                        S.op("gpsimd", lambda e, kc=kc, c0=c0: e.dma_start(out=wq[:, kc, c0:c0 + 1024], in_=peer_w_q[kc * 128:(kc + 1) * 128, c0:c0 + 1024]),
                             writes=["wq"], dma="wq")
                S.op("gpsimd", lambda e: e.dma_start(out=skn[:], in_=peer_sub_keys.rearrange("j k d -> k j d")), writes=["skn"], dma="skn")
                S.op("gpsimd", lambda e: e.dma_start(out=identb[:], in_=cst["ident"]), writes=["identb"], dma="identb3")
                S.op("sync", lambda e: e.dma_start(out=iota16[:], in_=cst["iota16"]), writes=["iota16"], dma="iota16")
                for k_, p_ in enumerate((ln1_g, ln1_b, ln2_g, ln2_b)):
                    S.op("sync", lambda e, k_=k_, p_=p_: e.dma_start(out=lnp[:, k_, :], in_=p_[0].partition_broadcast(128)), writes=["lnp"], dma="lnp")
                S.op("vector", lambda e: e.memset(epsb[:], EPS), writes=["epsb"])
                S.op("vector", lambda e: e.tensor_scalar(out=thr16[:], in0=iota16[:], scalar1=16.0, scalar2=None, op0=ALU.mult), reads=["iota16"], writes=["thr16"])
                for g0 in range(0, 16, 8):
                    for k in range(8):
                        S.op("tensor", lambda e, g0=g0, k=k: e.transpose(out=psb(0)[:, k * 128:(k + 1) * 128], in_=skn[:, g0 + k, :], identity=identb[:]),
                             reads=["skn", "identb"], writes=["ps0"])
                    S.op("vector", lambda e, g0=g0: e.tensor_copy(out=skT[:, g0:g0 + 8, :].rearrange("p a b -> p (a b)"), in_=psb(0)), reads=["ps0"], writes=["skT"])

            for i in range(G0, min(G0 + GRP[2], NT, NTL, NTL3)):
                t0 = i * 128
                b = i % 2
                S.op("sync", lambda e, b=b, t0=t0: e.dma_start(out=mixT[b][:], in_=mixT_d[t0 // 128]),
                     writes=[f"mixT{b}"], dma=f"mixT{b}")
                S.op("sync", lambda e, b=b, t0=t0: e.dma_start(out=xt[b][:], in_=x[t0:t0 + 128, :]), writes=[f"xt{b}"], dma=f"xt{b}")
                for nh in range(2):
                    for kc in range(8):
                        S.op("tensor", lambda e, nh=nh, kc=kc, b=b: e.matmul(ps[nh][:], lhsT=mixT[b][:, kc, :], rhs=wout[:, kc, nh * 512:(nh + 1) * 512],
                                                                             start=(kc == 0), stop=(kc == 7)),
                             reads=[f"mixT{b}", "wout"], writes=[f"ps{nh}"])
                    S.op("vector", lambda e, nh=nh, b=b: e.scalar_tensor_tensor(out=hpre[:, nh * 512:(nh + 1) * 512], in0=xt[b][:, nh * 512:(nh + 1) * 512], scalar=ALPHA,
                                                                                in1=ps[nh][:], op0=ALU.mult, op1=ALU.add),
                         reads=[f"xt{b}", f"ps{nh}"], writes=["hpre"])
                layer_norm(hpre, hres, 0, "a")
                if STOP3 <= 1:
                    continue
                S.op("scalar", lambda e: e.activation(out=hb[:], in_=hres[:], func=AF.Copy), reads=["hres"], writes=["hb"])
                for kc in range(8):
                    S.op("tensor", lambda e, kc=kc: e.transpose(out=psb(0)[:, kc * 128:(kc + 1) * 128], in_=hb[:, kc * 128:(kc + 1) * 128], identity=identb[:]),
                         reads=["hb", "identb"], writes=["ps0"])
                S.op("vector", lambda e: e.tensor_copy(out=hT[:].rearrange("p a b -> p (a b)"), in_=psb(0)), reads=["ps0"], writes=["hT"])
                for j in range(16):
                    pk = 4 + j // 4
                    for kc in range(8):
                        S.op("tensor", lambda e, j=j, kc=kc, pk=pk: e.matmul(ps[pk][:, (j % 4) * 128:(j % 4 + 1) * 128], lhsT=wq[:, kc, j * 128:(j + 1) * 128], rhs=hT[:, kc, :],
                                                                             start=(kc == 0), stop=(kc == 7)),
                             reads=["wq", "hT"], writes=[f"ps{pk}"])
                for q4 in range(4):
                    eng = "scalar" if q4 % 2 == 0 else "vector"
                    if eng == "scalar":
                        S.op("scalar", lambda e, q4=q4: e.activation(out=qTs[:, q4 * 4:(q4 + 1) * 4, :].rearrange("p a b -> p (a b)"), in_=ps[4 + q4][:], func=AF.Copy),
                             reads=[f"ps{4 + q4}"], writes=["qTs"])
                    else:
                        S.op("vector", lambda e, q4=q4: e.tensor_copy(out=qTs[:, q4 * 4:(q4 + 1) * 4, :].rearrange("p a b -> p (a b)"), in_=ps[4 + q4][:]),
                             reads=[f"ps{4 + q4}"], writes=["qTs"])
                for j in range(16):
                    pk = j // 4
                    S.op("tensor", lambda e, j=j, pk=pk: e.matmul(ps[pk][:, (j % 4) * 128:(j % 4 + 1) * 128], lhsT=qTs[:, j, :], rhs=skT[:, j, :], start=True, stop=True),
                         reads=["qTs", "skT"], writes=[f"ps{pk}"])
                for q4 in range(4):
                    if q4 % 2 == 0:
                        S.op("scalar", lambda e, q4=q4: e.activation(out=ssb[:, q4 * 4:(q4 + 1) * 4, :].rearrange("p a b -> p (a b)"), in_=ps[q4][:], func=AF.Copy),
                             reads=[f"ps{q4}"], writes=["ssb"])
                    else:
                        S.op("vector", lambda e, q4=q4: e.tensor_copy(out=ssb[:, q4 * 4:(q4 + 1) * 4, :].rearrange("p a b -> p (a b)"), in_=ps[q4][:]),
                             reads=[f"ps{q4}"], writes=["ssb"])
                if STOP3 <= 2:
                    continue
                for j in range(16):
                    S.op("vector", lambda e, j=j: e.max(out=tv[:, j, 0:8], in_=ssb[:, j, :]), reads=["ssb"], writes=["tv"])
                    S.op("vector", lambda e, j=j: e.max_index(out=ti[:, j, 0:8], in_max=tv[:, j, 0:8], in_values=ssb[:, j, :]), reads=["ssb", "tv"], writes=["ti"])
                    S.op("vector", lambda e, j=j: e.match_replace(out=ss2[:, j, :], in_to_replace=tv[:, j, 0:8], in_values=ssb[:, j, :], imm_value=NEG),
                         reads=["ssb", "tv"], writes=["ss2"])
                    S.op("vector", lambda e, j=j: e.max(out=tv[:, j, 8:16], in_=ss2[:, j, :]), reads=["ss2"], writes=["tv"])
                    S.op("vector", lambda e, j=j: e.max_index(out=ti[:, j, 8:16], in_max=tv[:, j, 8:16], in_values=ss2[:, j, :]), reads=["ss2", "tv"], writes=["ti"])
                S.op("vector", lambda e: e.tensor_copy(out=tif[:], in_=ti[:]), reads=["ti"], writes=["tif"])
                S.op("vector", lambda e: e.tensor_tensor(out=cand[:].rearrange("p h (a b) -> p h a b", b=16),
                                                        in0=tv[:, 0:16:2, :].unsqueeze(3).to_broadcast([128, 8, 16, 16]),
                                                        in1=tv[:, 1:16:2, :].unsqueeze(2).to_broadcast([128, 8, 16, 16]), op=ALU.add),
                     reads=["tv"], writes=["cand"])
                for h in range(8):
                    S.op("vector", lambda e, h=h: e.max(out=best[:, h, 0:8], in_=cand[:, h, :]), reads=["cand"], writes=["best"])
                    S.op("vector", lambda e, h=h: e.max_index(out=posu[:, h, 0:8], in_max=best[:, h, 0:8], in_values=cand[:, h, :]), reads=["cand", "best"], writes=["posu"])
                    S.op("vector", lambda e, h=h: e.match_replace(out=cand2[:, h, :], in_to_replace=best[:, h, 0:8], in_values=cand[:, h, :], imm_value=NEG),
                         reads=["cand", "best"], writes=["ss2"])
                    S.op("vector", lambda e, h=h: e.max(out=best[:, h, 8:16], in_=cand2[:, h, :]), reads=["ss2"], writes=["best"])
                    S.op("vector", lambda e, h=h: e.max_index(out=posu[:, h, 8:16], in_max=best[:, h, 8:16], in_values=cand2[:, h, :]), reads=["ss2", "best"], writes=["posu"])
                S.op("vector", lambda e: e.tensor_copy(out=posf[:], in_=posu[:]), reads=["posu"], writes=["posf"])
                S.op("vector", lambda e: e.tensor_tensor(out=big[:], in0=posf[:].unsqueeze(3).to_broadcast([128, 8, 16, 16]),
                                                        in1=thr16[:].unsqueeze(1).unsqueeze(1).to_broadcast([128, 8, 16, 16]), op=ALU.is_ge),
                     reads=["posf", "thr16"], writes=["ssb"])
                S.op("vector", lambda e: e.tensor_reduce(out=af[:], in_=big[:], axis=AX.X, op=ALU.add), reads=["ssb"], writes=["af"])
                S.op("vector", lambda e: e.tensor_scalar(out=af[:], in0=af[:], scalar1=-1.0, scalar2=None, op0=ALU.add), reads=["af"], writes=["af"])
                S.op("vector", lambda e: e.scalar_tensor_tensor(out=bf_[:], in0=af[:], scalar=-16.0, in1=posf[:], op0=ALU.mult, op1=ALU.add),
                     reads=["af", "posf"], writes=["bf_"])
                for (src, half, dst) in ((af, 0, ea), (bf_, 1, eb2)):
                    S.op("vector", lambda e, src=src: e.tensor_tensor(out=big[:], in0=src[:].unsqueeze(3).to_broadcast([128, 8, 16, 16]),
                                                                      in1=iota16[:].unsqueeze(1).unsqueeze(1).to_broadcast([128, 8, 16, 16]), op=ALU.is_equal),
                         reads=[src.name, "iota16"], writes=["ssb"])
                    S.op("vector", lambda e, half=half: e.tensor_tensor(out=big[:], in0=big[:],
                                                                        in1=tif[:, half:16:2, :].unsqueeze(2).to_broadcast([128, 8, 16, 16]), op=ALU.mult),
                         reads=["ssb", "tif"], writes=["ssb"])
                    S.op("vector", lambda e, dst=dst: e.tensor_reduce(out=dst[:], in_=big[:], axis=AX.X, op=ALU.add), reads=["ssb"], writes=[dst.name])
                S.op("vector", lambda e: e.scalar_tensor_tensor(out=ea[:], in0=ea[:], scalar=128.0, in1=eb2[:], op0=ALU.mult, op1=ALU.add),
                     reads=["ea", "eb2"], writes=["ea"])
                S.op("vector", lambda e: e.tensor_copy(out=eidx[:], in_=ea[:].rearrange("p h r -> p (h r)")), reads=["ea"], writes=["eidx"])
                S.op("vector", lambda e: e.tensor_tensor(out=gg[:], in0=best[:], in1=best[:, :, 0:1].to_broadcast([128, 8, 16]), op=ALU.subtract),
                     reads=["best"], writes=["gg"])
                S.op("scalar", lambda e: e.activation(out=gg[:], in_=gg[:], func=AF.Exp), reads=["gg"], writes=["gg"])
                S.op("vector", lambda e: e.tensor_reduce(out=gs[:], in_=gg[:], axis=AX.X, op=ALU.add), reads=["gg"], writes=["gs"])
                S.op("vector", lambda e: e.reciprocal(out=gs[:], in_=gs[:]), reads=["gs"], writes=["gs"])
                S.op("vector", lambda e: e.tensor_tensor(out=gg[:], in0=gg[:], in1=gs[:].unsqueeze(2).to_broadcast([128, 8, 16]), op=ALU.mult),
                     reads=["gg", "gs"], writes=["gg"])
                if STOP3 <= 3:
                    continue
                for sl in range(128):
                    k = sl % NB
                    S.op("gpsimd", lambda e, k=k, sl=sl: e.indirect_dma_start(out=ub[k][:], out_offset=None, in_=peer_u,
                                                                              in_offset=bass.IndirectOffsetOnAxis(ap=eidx[:, sl:sl + 1], axis=0)),
                         reads=["eidx"], writes=[f"ub{k}"], dma=f"ub{k}")
                    S.op("vector", lambda e, k=k, sl=sl: e.scalar_tensor_tensor(out=junk[:], in0=ub[k][:], scalar=1.0, in1=hres[:], op0=ALU.mult, op1=ALU.mult,
                                                                                accum_out=hd[:, sl:sl + 1]),
                         reads=[f"ub{k}", "hres"], writes=["junk", "hd"])
                if STOP3 <= 4:
                    continue
                S.op("vector", lambda e: e.tensor_tensor(out=g1[:], in0=hd[:], in1=hd[:], op=ALU.mult), reads=["hd"], writes=["g1"])
                S.op("vector", lambda e: e.tensor_scalar(out=g1[:], in0=g1[:], scalar1=0.044715, scalar2=1.0, op0=ALU.mult, op1=ALU.add), reads=["g1"], writes=["g1"])
                S.op("vector", lambda e: e.tensor_tensor(out=g1[:], in0=g1[:], in1=hd[:], op=ALU.mult), reads=["g1", "hd"], writes=["g1"])
                S.op("scalar", lambda e: e.activation(out=g2[:], in_=g1[:], func=AF.Tanh, scale=0.7978845608028654), reads=["g1"], writes=["g2"])
                S.op("vector", lambda e: e.tensor_scalar(out=g2[:], in0=g2[:], scalar1=1.0, scalar2=0.5, op0=ALU.add, op1=ALU.mult), reads=["g2"], writes=["g2"])
                S.op("vector", lambda e: e.tensor_tensor(out=g2[:], in0=g2[:], in1=hd[:], op=ALU.mult), reads=["g2", "hd"], writes=["g2"])
                S.op("vector", lambda e: e.tensor_tensor(out=wgt[:], in0=g2[:], in1=gg[:].rearrange("p h r -> p (h r)"), op=ALU.mult), reads=["g2", "gg"], writes=["wgt"])
                for sl in range(128):
                    k = sl % NB
                    S.op("gpsimd", lambda e, k=k, sl=sl: e.indirect_dma_start(out=ub[k][:], out_offset=None, in_=peer_v,
                                                                              in_offset=bass.IndirectOffsetOnAxis(ap=eidx[:, sl:sl + 1], axis=0)),
                         reads=["eidx"], writes=[f"ub{k}"], dma=f"ub{k}")
                    if sl == 0:
                        S.op("vector", lambda e, k=k: e.tensor_scalar(out=acc[:], in0=ub[k][:], scalar1=wgt[:, 0:1], scalar2=None, op0=ALU.mult),
                             reads=[f"ub{k}", "wgt"], writes=["hpre"])
                    else:
                        S.op("vector", lambda e, k=k, sl=sl: e.scalar_tensor_tensor(out=acc[:], in0=ub[k][:], scalar=wgt[:, sl:sl + 1], in1=acc[:], op0=ALU.mult, op1=ALU.add),
                             reads=[f"ub{k}", "wgt", "hpre"], writes=["hpre"])
                if STOP3 <= 5:
                    continue
                S.op("vector", lambda e: e.scalar_tensor_tensor(out=acc[:], in0=hres[:], scalar=ALPHA, in1=acc[:], op0=ALU.mult, op1=ALU.add),
                     reads=["hres", "hpre"], writes=["hpre"])
                layer_norm(acc, yo, 1, "b")
                S.op("sync", lambda e, t0=t0: e.dma_start(out=out[t0:t0 + 128, :], in_=yo[:]), reads=["yo"], writes=["d_out"], dma="st_out")
            S.final_wait("sync", ["d_out"])
            S.emit(block)
    return nc


def core_inputs(inputs, b, L):
    m = {"x": np.ascontiguousarray(inputs["x"][b, :L])}
    for k in ("w_in", "kv_norm_g", "w_uk", "w_uv", "hg_norm_g", "w_out", "ln1_g", "ln1_b", "peer_w_q",
              "peer_u", "peer_v", "ln2_g", "ln2_b"):
        m[k] = np.ascontiguousarray(inputs[k][0])
    m["hg_lb_logits"] = np.ascontiguousarray(inputs["hg_lb_logits"])
    m["peer_sub_keys"] = np.ascontiguousarray(inputs["peer_sub_keys"][0].reshape(16, 128, 128))
    for k, v in host_consts().items():
        m["c_" + k] = v
    return m


_CACHE = {}


def kernel(**inputs):
    L = inputs["x"].shape[1]
    if L not in _CACHE:
        _CACHE[L] = build(L)
    nc = _CACHE[L]
    outs = []
    for b in range(inputs["x"].shape[0]):
        res = run_bass_kernel_spmd(nc, [core_inputs(inputs, b, L)], core_ids=[0])
        outs.append(np.asarray(res.results[0]["out"], dtype=np.float32))
    return np.stack(outs, axis=0)
```
